# Optimizing a Trainium2 kernel written in Bass

```python
import math
import jax
import jax.numpy as jnp
from jax import lax
import numpy as np

D_MODEL = 2048
BATCH = 16
SEQ = 2048
DEPTH = 2

N_MEM = 256
XATTN_HEADS = 4
XATTN_DIM = D_MODEL // XATTN_HEADS

GDN_DIM = 128
GDN_HEADS = (3 * D_MODEL) // (8 * GDN_DIM)
GDN_WIDTH = GDN_HEADS * GDN_DIM
CONV_K = 4
GDN_CHUNK = 64

RET_DIM = 128
RET_HEADS = D_MODEL // (4 * RET_DIM)
RET_WIDTH = RET_HEADS * RET_DIM
RET_CHUNK = 64
ROPE_BASE = 10000.0

RWKV_DIM = 64
RWKV_HEADS = (3 * D_MODEL) // (8 * RWKV_DIM)
RWKV_WIDTH = RWKV_HEADS * RWKV_DIM
W_LORA = 64
A_LORA = 64
G_LORA = 128
RWKV_LNX_EPS = 64e-5

MIX_WIDTH = GDN_WIDTH + RET_WIDTH + RWKV_WIDTH
RWKV_IN = 3 * RWKV_WIDTH + W_LORA + A_LORA + G_LORA
RWKV_SPLITS = tuple(int(s) for s in np.cumsum((RWKV_WIDTH, RWKV_WIDTH, RWKV_WIDTH, W_LORA, A_LORA)))
IN_SIZES = (3 * GDN_WIDTH, GDN_WIDTH, GDN_HEADS, GDN_HEADS, 4 * RET_WIDTH, RWKV_IN)
IN_SPLITS = tuple(int(s) for s in np.cumsum(IN_SIZES)[:-1])
IN_WIDTH = int(sum(IN_SIZES))

FFN_HIDDEN = ((8 * D_MODEL + 3 * 256 - 1) // (3 * 256)) * 256

DEEPNORM_ALPHA = (2 * DEPTH) ** 0.25
DEEPNORM_BETA = (8 * DEPTH) ** -0.25

kernel_name = 'hybrid_gdn_retention_rwkv7_deepnorm_block'

F32 = jnp.float32


def layer_norm(x, g, b, eps=1e-5):
    xf = x.astype(F32)
    mu = jnp.mean(xf, -1, keepdims=True)
    var = jnp.mean(jnp.square(xf - mu), -1, keepdims=True)
    return ((xf - mu) * lax.rsqrt(var + eps) * g.astype(F32) + b.astype(F32)).astype(x.dtype)


def head_norm(y, eps):
    mu = jnp.mean(y, -1, keepdims=True)
    var = jnp.mean(jnp.square(y - mu), -1, keepdims=True)
    return (y - mu) * lax.rsqrt(var + eps)


def l2_normalize(y, eps=1e-6):
    return y * lax.rsqrt(jnp.sum(jnp.square(y), -1, keepdims=True) + eps)


def causal_conv(x, w):
    return lax.conv_general_dilated(
        x, w[:, None, :].astype(x.dtype), window_strides=(1,), padding=[(w.shape[0] - 1, 0)],
        dimension_numbers=('NWC', 'WIO', 'NWC'), feature_group_count=x.shape[-1])


def to_chunks(y, chunk):
    b, t, h, d = y.shape
    return y.reshape(b, t // chunk, chunk, h, d).transpose(1, 0, 3, 2, 4)


def from_chunks(y):
    n, b, h, c, d = y.shape
    return y.transpose(1, 0, 3, 2, 4).reshape(b, n * c, h, d)


def rotary(y, positions):
    d = y.shape[-1]
    inv_freq = ROPE_BASE ** (-jnp.arange(0, d, 2, dtype=F32) / d)
    ang = positions.astype(F32)[..., None] * inv_freq
    cos, sin = jnp.cos(ang)[:, :, None, :], jnp.sin(ang)[:, :, None, :]
    y1, y2 = jnp.split(y, 2, axis=-1)
    return jnp.concatenate([y1 * cos - y2 * sin, y2 * cos + y1 * sin], -1)


def gated_deltanet(qkv, gate, beta_logit, a_logit, conv_w, a_log, dt_bias, norm_w):
    b, t, _ = qkv.shape
    h, d, c = GDN_HEADS, GDN_DIM, GDN_CHUNK
    qkv = jax.nn.silu(causal_conv(qkv, conv_w)).astype(F32)
    q, k, v = (y.reshape(b, t, h, d) for y in jnp.split(qkv, 3, axis=-1))
    q = l2_normalize(q) * d ** -0.5
    k = l2_normalize(k)
    beta = jax.nn.sigmoid(beta_logit.astype(F32))
    log_a = -jnp.exp(a_log.astype(F32)) * jax.nn.softplus(a_logit.astype(F32) + dt_bias.astype(F32))
    qc, kc, vc = to_chunks(q, c), to_chunks(k, c), to_chunks(v, c)
    bc = to_chunks(beta[..., None], c)[..., 0]
    cum = jnp.cumsum(to_chunks(log_a[..., None], c)[..., 0], axis=-1)
    causal = jnp.tril(jnp.ones((c, c), bool))
    strict = jnp.tril(jnp.ones((c, c), bool), -1)
    diff = cum[..., :, None] - cum[..., None, :]
    seg = jnp.where(causal, jnp.exp(jnp.where(causal, diff, 0.0)), 0.0)
    kb = kc * bc[..., None]
    lhs = jnp.eye(c, dtype=F32) + jnp.where(strict, jnp.einsum('nbhid,nbhjd->nbhij', kb, kc) * seg, 0.0)
    u = lax.linalg.triangular_solve(lhs, vc * bc[..., None], left_side=True, lower=True, unit_diagonal=True)
    w = lax.linalg.triangular_solve(lhs, kb * jnp.exp(cum)[..., None], left_side=True, lower=True, unit_diagonal=True)
    qk = jnp.einsum('nbhid,nbhjd->nbhij', qc, kc) * seg

    def step(S, inp):
        q_i, k_i, u_i, w_i, cum_i, qk_i = inp
        v_new = u_i - jnp.einsum('bhck,bhkv->bhcv', w_i, S)
        o = (jnp.einsum('bhck,bhkv->bhcv', q_i * jnp.exp(cum_i)[..., None], S)
             + jnp.einsum('bhij,bhjv->bhiv', qk_i, v_new))
        last = cum_i[..., -1:]
        S = (S * jnp.exp(last)[..., None]
             + jnp.einsum('bhck,bhcv->bhkv', k_i * jnp.exp(last - cum_i)[..., None], v_new))
        return S, o

    _, o = lax.scan(step, jnp.zeros((b, h, d, d), F32), (qc, kc, u, w, cum, qk))
    o = from_chunks(o)
    o = o * lax.rsqrt(jnp.mean(jnp.square(o), -1, keepdims=True) + 1e-6) * norm_w.astype(F32)
    o = o * jax.nn.silu(gate.astype(F32)).reshape(b, t, h, d)
    return o.reshape(b, t, h * d)


def retention(qkvg, positions):
    b, t, _ = qkvg.shape
    h, d, c = RET_HEADS, RET_DIM, RET_CHUNK
    q, k, v, g = jnp.split(qkvg.astype(F32), 4, axis=-1)
    q = rotary(q.reshape(b, t, h, d), positions)
    k = rotary(k.reshape(b, t, h, d), positions) * d ** -0.5
    v = v.reshape(b, t, h, d)
    log_gamma = jnp.log1p(-jnp.exp2(-5.0 - jnp.arange(h, dtype=F32)))
    pos = jnp.arange(c, dtype=F32)
    rel = pos[:, None] - pos[None, :]
    decay_mask = jnp.where(rel >= 0, jnp.exp(jnp.where(rel >= 0, rel, 0.0) * log_gamma[:, None, None]), 0.0)
    qc, kc, vc = to_chunks(q, c), to_chunks(k, c), to_chunks(v, c)
    intra = jnp.einsum('nbhij,nbhjv->nbhiv', jnp.einsum('nbhid,nbhjd->nbhij', qc, kc) * decay_mask, vc)
    q_decay = jnp.exp((pos + 1.0) * log_gamma[:, None])
    k_decay = jnp.exp((c - 1.0 - pos) * log_gamma[:, None])
    chunk_decay = jnp.exp(c * log_gamma)

    def step(R, inp):
        q_i, k_i, v_i = inp
        o = jnp.einsum('bhck,bhkv->bhcv', q_i * q_decay[:, :, None], R)
        R = R * chunk_decay[:, None, None] + jnp.einsum('bhck,bhcv->bhkv', k_i * k_decay[:, :, None], v_i)
        return R, o

    _, inter = lax.scan(step, jnp.zeros((b, h, d, d), F32), (qc, kc, vc))
    y = head_norm(from_chunks(intra + inter), 1e-6)
    return jax.nn.silu(g) * y.reshape(b, t, h * d)


def rwkv7_time_mix(p, mu, w_up, w0, a_up, a0, g_up, k_k, k_a, r_k, lnx_w, lnx_b):
    b, t, _ = p.shape
    h, n = RWKV_HEADS, RWKV_DIM
    p = p.astype(F32)
    p_prev = jnp.pad(p, ((0, 0), (1, 0), (0, 0)))[:, :-1]
    p = p + (p_prev - p) * mu.astype(F32)
    r, k, v, wd, ad, gd = jnp.split(p, RWKV_SPLITS, axis=-1)
    w_log = -jax.nn.softplus(-(w0.astype(F32) + jnp.tanh(wd) @ w_up.astype(F32))) - 0.5
    decay = jnp.exp(-jnp.exp(w_log))
    a = jax.nn.sigmoid(a0.astype(F32) + ad @ a_up.astype(F32))
    g = jax.nn.sigmoid(gd) @ g_up.astype(F32)

    def heads(y):
        return y.reshape(b, t, h, n)

    kk = l2_normalize(heads(k * k_k.astype(F32)), 1e-12)
    k = k * (1.0 + (a - 1.0) * k_a.astype(F32))
    r, k, v, decay, a = heads(r), heads(k), heads(v), heads(decay), heads(a)

    def step(S, inp):
        r_t, w_t, k_t, v_t, a_t, b_t = inp
        sa = jnp.einsum('bhij,bhj->bhi', S, a_t)
        S = S * w_t[:, :, None, :] + sa[..., None] * b_t[:, :, None, :] + v_t[..., None] * k_t[:, :, None, :]
        return S, jnp.einsum('bhij,bhj->bhi', S, r_t)

    seq_major = [jnp.swapaxes(y, 0, 1) for y in (r, decay, k, v, -kk, kk * a)]
    _, y = lax.scan(step, jnp.zeros((b, h, n, n), F32), tuple(seq_major))
    y = jnp.swapaxes(y, 0, 1)
    y = head_norm(y, RWKV_LNX_EPS) * lnx_w.astype(F32).reshape(h, n) + lnx_b.astype(F32).reshape(h, n)
    y = y + jnp.sum(r * k * r_k.astype(F32).reshape(h, n), -1, keepdims=True) * v
    return y.reshape(b, t, h * n) * g


def hybrid_mixer(x, positions, w_in, gdn_conv, gdn_a_log, gdn_dt_bias, gdn_norm,
                 rwkv_mu, rwkv_w_up, rwkv_w0, rwkv_a_up, rwkv_a0, rwkv_g_up,
                 rwkv_k_k, rwkv_k_a, rwkv_r_k, rwkv_lnx_w, rwkv_lnx_b, w_out):
    gdn_qkv, gdn_gate, gdn_beta, gdn_alpha, ret_qkvg, rwkv_in = jnp.split(x @ w_in, IN_SPLITS, axis=-1)
    y_a = gated_deltanet(gdn_qkv, gdn_gate, gdn_beta, gdn_alpha, gdn_conv, gdn_a_log, gdn_dt_bias, gdn_norm)
    y_b = retention(ret_qkvg, positions)
    y_c = rwkv7_time_mix(rwkv_in, rwkv_mu, rwkv_w_up, rwkv_w0, rwkv_a_up, rwkv_a0, rwkv_g_up,
                         rwkv_k_k, rwkv_k_a, rwkv_r_k, rwkv_lnx_w, rwkv_lnx_b)
    y = jnp.concatenate([y_a, y_b, y_c], axis=-1).astype(x.dtype)
    return y @ w_out


def memory_cross_attention(x, mem, wq, wk, wv, wo):
    b, t, _ = x.shape
    m = mem.shape[1]
    q = (x @ wq).reshape(b, t, XATTN_HEADS, XATTN_DIM)
    k = (mem @ wk).reshape(b, m, XATTN_HEADS, XATTN_DIM)
    v = (mem @ wv).reshape(b, m, XATTN_HEADS, XATTN_DIM)
    s = jnp.einsum('bthd,bmhd->bhtm', q, k).astype(F32) * XATTN_DIM ** -0.5
    probs = jax.nn.softmax(s, axis=-1).astype(v.dtype)
    o = jnp.einsum('bhtm,bmhd->bthd', probs, v).reshape(b, t, XATTN_HEADS * XATTN_DIM)
    return o @ wo


def swiglu_ffn(x, w_gate_up, w_down):
    gate, up = jnp.split(x @ w_gate_up, 2, axis=-1)
    return (jax.nn.silu(gate) * up) @ w_down


def setup_inputs(seed: int = 0) -> dict:
    key = jax.random.key(seed)
    keys = jax.random.split(key, 32)
    L = DEPTH

    def normal(i, shape, scale):
        return scale * jax.random.normal(keys[i], shape, F32)

    def uniform(i, shape, lo, hi):
        return jax.random.uniform(keys[i], shape, F32, lo, hi)

    x = normal(0, (BATCH, SEQ, D_MODEL), 1.0)
    mem = normal(1, (BATCH, N_MEM, D_MODEL), 1.0)
    offset = jax.random.randint(keys[2], (BATCH, 1), 0, 1024, dtype=jnp.int32)
    positions = jnp.arange(SEQ, dtype=jnp.int32)[None, :] + offset
    dt = jnp.exp(uniform(6, (L, GDN_HEADS), math.log(1e-3), math.log(1e-1)))
    return {
        'x': x,
        'mem': mem,
        'positions': positions,
        'w_in': normal(3, (L, D_MODEL, IN_WIDTH), D_MODEL ** -0.5),
        'gdn_conv': normal(4, (L, CONV_K, 3 * GDN_WIDTH), CONV_K ** -0.5),
        'gdn_a_log': jnp.log(uniform(5, (L, GDN_HEADS), 1.0, 16.0)),
        'gdn_dt_bias': dt + jnp.log(-jnp.expm1(-dt)),
        'gdn_norm': 1.0 + normal(7, (L, GDN_DIM), 0.02),
        'rwkv_mu': uniform(8, (L, RWKV_IN), 0.0, 1.0),
        'rwkv_w_up': normal(9, (L, W_LORA, RWKV_WIDTH), 0.1),
        'rwkv_w0': uniform(10, (L, RWKV_WIDTH), -6.0, 1.0),
        'rwkv_a_up': normal(11, (L, A_LORA, RWKV_WIDTH), 0.5 * A_LORA ** -0.5),
        'rwkv_a0': normal(12, (L, RWKV_WIDTH), 0.1),
        'rwkv_g_up': normal(13, (L, G_LORA, RWKV_WIDTH), G_LORA ** -0.5),
        'rwkv_k_k': 0.85 + normal(14, (L, RWKV_WIDTH), 0.02),
        'rwkv_k_a': 1.0 + normal(15, (L, RWKV_WIDTH), 0.02),
        'rwkv_r_k': normal(16, (L, RWKV_WIDTH), 0.1),
        'rwkv_lnx_w': 1.0 + normal(17, (L, RWKV_WIDTH), 0.02),
        'rwkv_lnx_b': normal(18, (L, RWKV_WIDTH), 0.02),
        'w_out': normal(19, (L, MIX_WIDTH, D_MODEL), DEEPNORM_BETA * MIX_WIDTH ** -0.5),
        'ln1_g': 1.0 + normal(20, (L, D_MODEL), 0.02),
        'ln1_b': normal(21, (L, D_MODEL), 0.02),
        'xattn_q': normal(22, (L, D_MODEL, D_MODEL), D_MODEL ** -0.5),
        'xattn_k': normal(23, (L, D_MODEL, D_MODEL), D_MODEL ** -0.5),
        'xattn_v': normal(24, (L, D_MODEL, D_MODEL), D_MODEL ** -0.5),
        'xattn_o': normal(25, (L, D_MODEL, D_MODEL), DEEPNORM_BETA * D_MODEL ** -0.5),
        'ln2_g': 1.0 + normal(26, (L, D_MODEL), 0.02),
        'ln2_b': normal(27, (L, D_MODEL), 0.02),
        'ffn_gate_up': normal(28, (L, D_MODEL, 2 * FFN_HIDDEN), D_MODEL ** -0.5),
        'ffn_down': normal(29, (L, FFN_HIDDEN, D_MODEL), DEEPNORM_BETA * FFN_HIDDEN ** -0.5),
        'ln3_g': 1.0 + normal(30, (L, D_MODEL), 0.02),
        'ln3_b': normal(31, (L, D_MODEL), 0.02),
    }


def reference(x, mem, positions, w_in, gdn_conv, gdn_a_log, gdn_dt_bias, gdn_norm,
              rwkv_mu, rwkv_w_up, rwkv_w0, rwkv_a_up, rwkv_a0, rwkv_g_up,
              rwkv_k_k, rwkv_k_a, rwkv_r_k, rwkv_lnx_w, rwkv_lnx_b, w_out,
              ln1_g, ln1_b, xattn_q, xattn_k, xattn_v, xattn_o, ln2_g, ln2_b,
              ffn_gate_up, ffn_down, ln3_g, ln3_b):
    for l in range(DEPTH):
        mix = hybrid_mixer(x, positions, w_in[l], gdn_conv[l], gdn_a_log[l], gdn_dt_bias[l], gdn_norm[l],
                           rwkv_mu[l], rwkv_w_up[l], rwkv_w0[l], rwkv_a_up[l], rwkv_a0[l], rwkv_g_up[l],
                           rwkv_k_k[l], rwkv_k_a[l], rwkv_r_k[l], rwkv_lnx_w[l], rwkv_lnx_b[l], w_out[l])
        x = layer_norm(DEEPNORM_ALPHA * x + mix, ln1_g[l], ln1_b[l])
        xa = memory_cross_attention(x, mem, xattn_q[l], xattn_k[l], xattn_v[l], xattn_o[l])
        x = layer_norm(DEEPNORM_ALPHA * x + xa, ln2_g[l], ln2_b[l])
        x = layer_norm(DEEPNORM_ALPHA * x + swiglu_ffn(x, ffn_gate_up[l], ffn_down[l]), ln3_g[l], ln3_b[l])
    return x
```

```python
import math
import numpy as np
import concourse.bass as bass
import concourse.mybir as mybir
from concourse.bass_utils import run_bass_kernel_spmd

F32 = mybir.dt.float32
BF16 = mybir.dt.bfloat16
I32 = mybir.dt.int32
AF = mybir.ActivationFunctionType
ALU = mybir.AluOpType
AX = mybir.AxisListType

D = 2048
KC = 16
NMEM = 256
FFN = 5632
DEPTH = 2
ALPHA = (2 * DEPTH) ** 0.25
GQ, GK, GV, GG, GB, GA = 0, 768, 1536, 2304, 3072, 3078
RQ, RK, RV, RG = 3084, 3596, 4108, 4620
WR, WK, WV, WWD, WAD, WGD = 5132, 5900, 6668, 7436, 7500, 7564
INW = 7692
TWO_PI = 2.0 * math.pi
C1 = 6.28125
C2 = TWO_PI - C1


class Buf:
    def __init__(self, name, h):
        self.name = name
        self.h = h

    def __getitem__(self, idx):
        return self.h[idx]


class V:
    def __init__(self, buf, ap, sub=None):
        self.ap = ap
        if isinstance(sub, (list, tuple)):
            self.keys = [(buf.name, s_) for s_ in sub]
        else:
            self.keys = [(buf.name, sub)]


class Prog:
    LIM = 20000
    NDS = 40

    def __init__(self):
        self.nc = bass.Bass("TRN2", target_bir_lowering=False)
        nc = self.nc
        self.E = {"pe": nc.tensor, "act": nc.scalar, "dve": nc.vector, "pool": nc.gpsimd, "sp": nc.sync}
        self.cnt = {e: 0 for e in self.E}
        self.semh = {}
        self.seen = {e: {} for e in self.E}
        self.maxep = {e: {} for e in self.E}
        self.state = {}
        self.pending = {e: [] for e in self.E}
        self.dma_val = [0] * self.NDS
        self.dma_next = 0
        for i in range(self.NDS):
            self.semh[("d", i)] = nc.alloc_semaphore(f"dsem{i}")
        self.nbuf = 0
        self.taps = {}
        self.ninst = 0

    def sb(self, name, shape, dtype=F32):
        return Buf(name, self.nc.alloc_sbuf_tensor(name, list(shape), dtype))

    def psum(self, name):
        return Buf(name, self.nc.alloc_psum_tensor(name, [128, 512], F32))

    def dram(self, name, shape, dtype, kind):
        return self.nc.dram_tensor(name, list(shape), dtype, kind=kind).ap()

    def _sem(self, key):
        if key not in self.semh:
            self.semh[key] = self.nc.alloc_semaphore("s_%s_%d" % (key[1], key[2]))
        return self.semh[key]

    def _wait(self, eng, ev):
        if ev is None:
            return
        key, val = ev
        if key[0] == "e":
            if eng == "pe" and key[1] == "pe":
                return
            if self.maxep[eng].get(key[1], -1) > key[2]:
                return
        if self.seen[eng].get(key, 0) >= val:
            return
        self.E[eng].wait_ge(self._sem(key), val)
        self.ninst += 1
        self.seen[eng][key] = val
        if key[0] == "e":
            self.maxep[eng][key[1]] = max(self.maxep[eng].get(key[1], -1), key[2])

    def _st(self, key):
        name, sub = key
        d = self.state.setdefault(name, {})
        if sub not in d:
            d[sub] = {"w": None, "r": {}}
        return d[sub]

    def _deps(self, R, W, eng=None):
        deps = []
        for name, sub in R:
            d = self.state.setdefault(name, {})
            subs = list(d.keys()) if sub is None else [sub, None]
            for s2 in subs:
                if s2 in d:
                    deps.append(d[s2]["w"])
                    if name.startswith("ps"):
                        deps.extend(ev for rk, ev in d[s2]["r"].items() if rk != eng)
        for name, sub in W:
            d = self.state.setdefault(name, {})
            subs = list(d.keys()) if sub is None else [sub, None]
            for s2 in subs:
                if s2 in d:
                    deps.append(d[s2]["w"])
                    deps.extend(d[s2]["r"].values())
        return deps

    def _register(self, R, W, ev, rkey):
        for key in R:
            self._st(key)["r"][rkey] = ev
        for name, sub in W:
            if sub is None:
                self.state[name] = {None: {"w": ev, "r": {}}}
            else:
                st = self._st((name, sub))
                st["w"] = ev
                st["r"] = {}

    def op(self, eng, fn, ins, outs, count=True):
        R = [k_ for v in ins for k_ in v.keys]
        W = [k_ for v in outs for k_ in v.keys]
        for ev in self._deps(R, W, eng):
            self._wait(eng, ev)
        inst = fn(self.E[eng])
        self.ninst += 1
        if not count:
            self.pending[eng].append((R, W))
            return
        n = self.cnt[eng]
        self.cnt[eng] = n + 1
        key = ("e", eng, n // self.LIM)
        ev = (key, n % self.LIM + 1)
        inst.then_inc(self._sem(key), 1)
        for (r2, w2) in self.pending[eng]:
            self._register(r2, w2, ev, eng)
        self.pending[eng] = []
        self._register(R, W, ev, eng)

    def dma(self, q, out, in_, ins, outs):
        R = [k_ for v in ins for k_ in v.keys]
        W = [k_ for v in outs for k_ in v.keys]
        half = self.NDS // 2
        if not hasattr(self, "dma_nq"):
            self.dma_nq = {}
        j = self.dma_nq.get(q, 0)
        self.dma_nq[q] = (j + 1) % half
        i = j + (0 if q == "pool" else half)
        key = ("d", i)
        if self.dma_val[i] > 0:
            self._wait(q, (key, self.dma_val[i]))
        for ev in self._deps(R, W):
            self._wait(q, ev)
        inst = self.E[q].dma_start(out=out, in_=in_)
        self.ninst += 1
        self.dma_val[i] += 16
        ev = (key, self.dma_val[i])
        inst.then_inc(self.semh[key], 16)
        self._register(R, W, ev, key)

    def _latest(self):
        evs = []
        for e in ("pe", "act", "dve", "pool"):
            n = self.cnt[e]
            if n > 0:
                evs.append((("e", e, (n - 1) // self.LIM), (n - 1) % self.LIM + 1))
        for i in range(self.NDS):
            if self.dma_val[i] > 0:
                evs.append((("d", i), self.dma_val[i]))
        return evs

    def barrier(self):
        evs = self._latest()
        for e in self.E:
            assert not self.pending[e]
            for ev in evs:
                if ev[0][0] == "e" and ev[0][1] == e:
                    continue
                self._wait(e, ev)
        self.state = {}

    def finish(self):
        for ev in self._latest():
            self._wait("sp", ev)

    def tt(self, eng, o, a, b, op):
        self.op(eng, lambda e: e.tensor_tensor(o.ap, a.ap, b.ap, op), [a, b], [o])

    def ts(self, eng, o, a, s1, s2, op0, op1=None, extra=()):
        if op1 is None:
            self.op(eng, lambda e: e.tensor_scalar(o.ap, a.ap, s1, None, op0), [a] + list(extra), [o])
        else:
            self.op(eng, lambda e: e.tensor_scalar(o.ap, a.ap, s1, s2, op0, op1), [a] + list(extra), [o])

    def stt(self, o, a, s, b, op0, op1, extra=()):
        self.op("dve", lambda e: e.scalar_tensor_tensor(o.ap, a.ap, s, b.ap, op0, op1), [a, b] + list(extra), [o])

    def act(self, o, a, func, bias=None, scale=None, accum=None, extra=()):
        kw = {}
        if bias is not None:
            kw["bias"] = bias
        if scale is not None:
            kw["scale"] = scale
        outs = [o]
        if accum is not None:
            kw["accum_out"] = accum.ap
            outs.append(accum)
        self.op("act", lambda e: e.activation(o.ap, a.ap, func, **kw), [a] + list(extra), outs)

    def copy(self, eng, o, a):
        if eng == "act":
            self.op("act", lambda e: e.copy(o.ap, a.ap), [a], [o])
        else:
            self.op(eng, lambda e: e.tensor_copy(o.ap, a.ap), [a], [o])

    def mm(self, o, lhsT, rhs, start, stop, count=None):
        if count is None:
            count = stop
        self.op("pe", lambda e: e.matmul(o.ap, lhsT.ap, rhs.ap, start=start, stop=stop), [lhsT, rhs], [o], count=count)

    def tr(self, o, a, ident):
        self.op("pe", lambda e: e.transpose(o.ap, a.ap, ident.ap), [a, ident], [o])

    def recip(self, o, a):
        self.op("dve", lambda e: e.reciprocal(o.ap, a.ap), [a], [o])

    def memset(self, eng, o, val):
        self.op(eng, lambda e: e.memset(o.ap, val), [], [o])

    def rsum(self, o, a):
        self.op("dve", lambda e: e.reduce_sum(o.ap, a.ap, AX.X), [a], [o])


MAGIC = 12582912.0
RET_GAMMA = [1.0 - 2.0 ** (-5.0 - h) for h in range(4)]
NCST = 17
NSTATE = 768 + 512 + 384 + 54 + 20


def make_consts():
    c = np.zeros((128, NCST, 128), np.float32)
    i = np.arange(128)
    c[:, 0, :] = np.eye(128)
    c[:, 1, :] = (i[:, None] <= i[None, :])
    c[:, 2, :] = (i[:, None] < i[None, :])
    c[:, 3, :] = (i[:, None] <= i[None, :])
    c[:, 4, :] = 1.0
    c[:, 5, :] = ((i[:, None] // 64) == (i[None, :] // 64))
    for p in range(128):
        c[(p + 64) % 128, 6, p] = -1.0 if p < 64 else 1.0
    inv_freq = (np.float32(10000.0) ** (-np.arange(0, 128, 2, dtype=np.float32) / np.float32(128))).astype(np.float32)
    c[:, 7, 0] = inv_freq[i % 64]
    c[:, 7, 1] = (i < 64)
    c[:, 7, 2] = (i >= 64)
    for h in range(4):
        lg = np.log1p(-np.exp2(np.float64(-5.0 - h)))
        c[:, 8 + h, :] = np.exp((i[None, :] + 1.0) * lg)
        c[:, 12 + h, :] = np.exp(-(i[None, :] + 1.0) * lg) * 128.0 ** -0.5
    return c


def build(nseq, T, NT, depth, taps=(), skip=(), state_io=False):
    P = Prog()
    NTT = NT // 128
    NST = T // NT
    taps = set(taps)

    def din(name, shape, dt=F32):
        return P.dram(name, shape, dt, "ExternalInput")

    x_d = din("x", [nseq, T, D])
    mem_d = din("mem", [nseq, NMEM, D])
    pos_d = din("positions", [nseq, T])
    w_in_d = din("w_in", [depth, D, INW])
    w_out_d = din("w_out", [depth, D, D])
    wq_d = din("xattn_q", [depth, D, D])
    wk_d = din("xattn_k", [depth, D, D])
    wv_d = din("xattn_v", [depth, D, D])
    wo_d = din("xattn_o", [depth, D, D])
    wgu_d = din("ffn_gate_up", [depth, D, 2 * FFN])
    wd_d = din("ffn_down", [depth, FFN, D])
    lnp_d = din("lnp", [depth, 6, D])
    conv_d = din("gdn_conv_t", [depth, 128, 18 * 4])
    gsm_d = din("gdn_small", [depth, 12])
    gnorm_d = din("gdn_norm", [depth, 128])
    mu_d = din("rwkv_mu_t", [depth, 128, 20])
    rvec_d = din("rwkv_vec_t", [depth, 128, 30])
    lnx_d = din("rwkv_lnx", [depth, 2 * 768])
    wup_d = din("rwkv_w_up", [depth, 64, 768])
    aup_d = din("rwkv_a_up", [depth, 64, 768])
    gup_d = din("rwkv_g_up", [depth, 128, 768])
    cst_d = din("consts", [128, NCST, 128])
    out_d = P.dram("out", [nseq, T, D], F32, "ExternalOutput")
    if state_io:
        stin_d = din("state_in", [nseq, 128, NSTATE])
        stout_d = P.dram("state_out", [nseq, 128, NSTATE], F32, "ExternalOutput")

    xres = P.sb("xres", [128, NTT, D])
    xT = P.sb("xT", [128, KC, NT], BF16)
    yT = P.sb("yT", [128, KC, NT], BF16)
    wb = [P.sb(f"wb{i}", [128, KC * 512], BF16) for i in range(2)]
    cst = P.sb("cst", [128, NCST, 128])
    small = P.sb("small", [128, 64])
    junk = P.sb("junk", [128, D], BF16)
    NS = 20
    SC = P.sb("SC", [128, NS, 512])
    memT = P.sb("memT", [128, KC, NMEM], BF16)
    qTh = P.sb("qTh", [128, 4, NT], BF16)
    kTh = P.sb("kTh", [128, 4, NMEM], BF16)
    vh = P.sb("vh", [128, 2, 512], BF16)
    Eh = P.sb("Eh", [128, 2, NT], BF16)
    onesb = P.sb("onesb", [128, 128], BF16)
    posi = P.sb("posi", [128, NT])
    PS = [P.psum(f"ps{i}") for i in range(8)]
    S_g = P.sb("S_g", [128, depth * 6, 128])
    S_r = P.sb("S_r", [128, depth * 4, 128])
    S_w = P.sb("S_w", [128, depth * 6, 64])
    cv_c = P.sb("cv_c", [128, depth * 18, 3])
    ts_c = P.sb("ts_c", [128, depth * 20])
    cbuf = P.sb("cbuf", [128, 3, NT + 4])
    convw = P.sb("convw", [128, 18 * 4])
    gsm = P.sb("gsm", [128, 12])
    gnorm = P.sb("gnorm", [128, 128])
    mu = P.sb("mu", [128, 20])
    rvec = P.sb("rvec", [128, 30])
    wup = P.sb("wup", [128, 768])
    aup = P.sb("aup", [128, 768])
    gup = P.sb("gup", [128, 768])
    gtm = P.sb("gtm", [128, NTT, 12])
    gL = P.sb("gL", [128, NTT, 12])

    pscnt = [0]

    def ps():
        i = pscnt[0] % 8
        pscnt[0] += 1
        return PS[i]

    ewc = [0]

    def ev_eng():
        ewc[0] += 1
        return "act" if ewc[0] % 2 else "dve"

    def W(b, ap=None, sub=None):
        return V(b, b[:] if ap is None else ap, sub)

    def C(i):
        return V(cst, cst[:, i, :])

    def Sq(slot, q0=0, nq=1, p0=0, p1=128):
        return V(SC, SC[p0:p1, slot, q0 * 128:(q0 + nq) * 128], [slot * 4 + q for q in range(q0, q0 + nq)])

    def Sl(slot, n=512):
        return V(SC, SC[:, slot, 0:n], [slot * 4 + q for q in range(4)])

    ident = C(0)
    tri = C(1)
    mstrict = C(2)
    mincl = C(3)
    ones = C(4)
    bones = C(5)
    psw = C(6)
    SM = V(small, small[:])

    def smc(i):
        return V(small, small[:, i:i + 1], i)

    def tap(name, view, shape):
        if name not in taps:
            return
        t = P.dram("tap_" + name, shape, F32, "ExternalOutput")
        for i_ in range(shape[1]):
            P.dma("pool", t[:, i_], view.ap[:, i_], [view], [])

    def load_w(wbuf, src2d, K, ncols, col_off=0, tot=None, q="pool"):
        kc = K // 128
        tot = ncols if tot is None else tot
        dst = wbuf[:, 0:kc * tot].rearrange("p (k n) -> p k n", k=kc)
        src = src2d.rearrange("(k p) n -> p k n", p=128)
        for k0 in range(0, kc, 4):
            k1 = min(kc, k0 + 4)
            P.dma(q, dst[:, k0:k1, col_off:col_off + ncols], src[:, k0:k1, :], [], [W(wbuf)])
        return dst

    def make_T(src_fn, dstT, ntiles, width):
        nb = width // 128
        for t_ in range(ntiles):
            for g in range(0, nb, 4):
                p_ = ps()
                for j in range(4):
                    P.tr(V(p_, p_[:, j * 128:(j + 1) * 128]), src_fn(t_, (g + j) * 128, (g + j + 1) * 128), ident)
                P.copy(ev_eng(), W(dstT, dstT[:, g:g + 4, t_ * 128:(t_ + 1) * 128]),
                       V(p_, p_[:].rearrange("p (a b) -> p a b", a=4)))

    def lin_W(wbuf, wv_, j, rhsT, nk, ntok):
        p_ = ps()
        for k in range(nk):
            P.mm(V(p_, p_[:, 0:ntok]), W(wbuf, wv_[:, k, j * 128:(j + 1) * 128]),
                 W(rhsT, rhsT[:, k, 0:ntok]), k == 0, k == nk - 1)
        return p_

    def lin_X(wbuf, wv_, c0, ncols, lhsT, nk, t_):
        p_ = ps()
        for k in range(nk):
            P.mm(V(p_, p_[:, 0:ncols]), W(lhsT, lhsT[:, k, t_ * 128:(t_ + 1) * 128]),
                 W(wbuf, wv_[:, k, c0:c0 + ncols]), k == 0, k == nk - 1)
        return p_

    wbi = [0]

    def next_wb():
        wbi[0] += 1
        return wb[wbi[0] % 2]

    def rsqrt_col(dst, src, eps_i, scale):
        P.act(smc(dst), smc(src), AF.Sqrt, bias=small[:, eps_i:eps_i + 1], scale=scale, extra=[smc(eps_i)])
        P.recip(smc(dst), smc(dst))

    def layer_norm(l, which):
        lnp = SC[:, 0:8, :].rearrange("p (j a) f -> p j (a f)", j=2)
        lk = [list(range(0, 16)), list(range(16, 32))]
        for j in range(2):
            P.dma("sp", lnp[:, j, :], lnp_d[l, 2 * which + j:2 * which + j + 1, :].to_broadcast([128, D]), [], [V(SC, None, lk[j])])
        for t_ in range(NTT):
            xv = W(xres, xres[:, t_, :], t_)
            P.rsum(smc(16), xv)
            P.ts("dve", smc(17), smc(16), -1.0 / D, None, ALU.mult)
            P.act(xv, xv, AF.Identity, bias=small[:, 17:18], extra=[smc(17)])
            P.act(W(junk), xv, AF.Square, accum=smc(18))
            rsqrt_col(19, 18, 0, 1.0 / D)
            P.stt(xv, xv, small[:, 19:20], V(SC, lnp[:, 0, :], lk[0]), ALU.mult, ALU.mult, extra=[smc(19)])
            P.tt("dve", xv, xv, V(SC, lnp[:, 1, :], lk[1]), ALU.add)

    def sublayer_out(w2d, l, which):
        for cb_ in range(4):
            wbuf = next_wb()
            wv_ = load_w(wbuf, w2d[:, cb_ * 512:(cb_ + 1) * 512], D, 512)
            for t_ in range(NTT):
                p_ = lin_X(wbuf, wv_, 0, 512, yT, KC, t_)
                xv = W(xres, xres[:, t_, cb_ * 512:(cb_ + 1) * 512], t_)
                P.stt(xv, xv, ALPHA, V(p_, p_[:, 0:512]), ALU.mult, ALU.add)
        layer_norm(l, which)

    def ffn(l):
        wgu, wd = wgu_d[l], wd_d[l]
        for hb in range(FFN // 512):
            wg_b = next_wb()
            wgv = load_w(wg_b, wgu[:, hb * 512:(hb + 1) * 512], D, 512)
            wu_b = next_wb()
            wuv = load_w(wu_b, wgu[:, FFN + hb * 512:FFN + (hb + 1) * 512], D, 512)
            for j in range(4):
                pg = lin_W(wg_b, wgv, j, xT, KC, NT)
                pu = lin_W(wu_b, wuv, j, xT, KC, NT)
                sg = Sl(8 + (j % 2), NT)
                P.act(sg, V(pg, pg[:, 0:NT]), AF.Sigmoid)
                P.tt("dve", sg, sg, V(pg, pg[:, 0:NT]), ALU.mult)
                P.tt("dve", W(yT, yT[:, j, 0:NT], j), sg, V(pu, pu[:, 0:NT]), ALU.mult)
            wd_b = next_wb()
            wdv = load_w(wd_b, wd[hb * 512:(hb + 1) * 512, :], 512, D)
            for cb_ in range(4):
                for t_ in range(NTT):
                    p_ = ps()
                    for k in range(4):
                        P.mm(V(p_, p_[:, 0:512]), W(yT, yT[:, k, t_ * 128:(t_ + 1) * 128], k),
                             W(wd_b, wdv[:, k, cb_ * 512:(cb_ + 1) * 512]), k == 0, k == 3)
                    xv = W(xres, xres[:, t_, cb_ * 512:(cb_ + 1) * 512], t_)
                    if hb == 0:
                        P.stt(xv, xv, ALPHA, V(p_, p_[:, 0:512]), ALU.mult, ALU.add)
                    else:
                        P.tt("dve", xv, xv, V(p_, p_[:, 0:512]), ALU.add)
        layer_norm(l, 2)

    def xattn(l, sq):
        mt_ = SC[:, 0:8, :].rearrange("p (j a) f -> p j (a f)", j=2)
        mk = list(range(0, 32))
        for j in range(2):
            P.dma("sp", mt_[:, j, :], mem_d[sq, j * 128:(j + 1) * 128, :], [], [V(SC, None, mk)])
        make_T(lambda t_, c0, c1: V(SC, mt_[:, t_, c0:c1], mk), memT, 2, D)
        for h in range(4):
            wbuf = next_wb()
            wv_ = load_w(wbuf, wq_d[l][:, h * 512:(h + 1) * 512], D, 512)
            for j in range(4):
                p_ = lin_W(wbuf, wv_, j, xT, KC, NT)
                P.act(W(qTh, qTh[:, j, :], j), V(p_, p_[:, 0:NT]), AF.Copy, scale=512.0 ** -0.5)
            wbuf = next_wb()
            wv_ = load_w(wbuf, wk_d[l][:, h * 512:(h + 1) * 512], D, 512)
            for j in range(4):
                p_ = lin_W(wbuf, wv_, j, memT, KC, NMEM)
                P.copy(ev_eng(), W(kTh, kTh[:, j, :], j), V(p_, p_[:, 0:NMEM]))
            wbuf = next_wb()
            wv_ = load_w(wbuf, wv_d[l][:, h * 512:(h + 1) * 512], D, 512)
            for m_ in range(2):
                p_ = lin_X(wbuf, wv_, 0, 512, memT, KC, m_)
                P.copy(ev_eng(), W(vh, vh[:, m_, :], m_), V(p_, p_[:, 0:512]))
            for m_ in range(2):
                p_ = ps()
                for j in range(4):
                    P.mm(V(p_, p_[:, 0:NT]), W(kTh, kTh[:, j, m_ * 128:(m_ + 1) * 128], j), W(qTh, qTh[:, j, :], j), j == 0, j == 3)
                P.act(W(Eh, Eh[:, m_, :], m_), V(p_, p_[:, 0:NT]), AF.Exp)
            pz = ps()
            for m_ in range(2):
                P.mm(V(pz, pz[:, 0:NT]), W(onesb), W(Eh, Eh[:, m_, :], m_), m_ == 0, m_ == 1)
            rz = Sl(10, NT)
            P.recip(rz, V(pz, pz[:, 0:NT]))
            for j in range(4):
                p_ = ps()
                for m_ in range(2):
                    P.mm(V(p_, p_[:, 0:NT]), W(vh, vh[:, m_, j * 128:(j + 1) * 128], m_), W(Eh, Eh[:, m_, :], m_), m_ == 0, m_ == 1)
                P.tt("dve", W(yT, yT[:, 4 * h + j, :], 4 * h + j), V(p_, p_[:, 0:NT]), rz, ALU.mult)

    def neumann(XT0, X0, Y, tmpA, tmpB, ncolsY):
        curT, cur = XT0, X0
        nxtT, nxt = tmpA, tmpB
        for lev in range(7):
            p_ = ps()
            pv = V(p_, p_[:, 0:ncolsY])
            P.mm(pv, curT, Y, True, True)
            P.tt("dve", Y, Y, pv, ALU.add)
            if lev == 6:
                break
            p1 = ps()
            P.mm(V(p1, p1[:, 0:128]), cur, curT, True, True)
            p2 = ps()
            P.mm(V(p2, p2[:, 0:128]), curT, cur, True, True)
            P.copy("act", nxtT, V(p1, p1[:, 0:128]))
            P.copy("act", nxt, V(p2, p2[:, 0:128]))
            curT, cur, nxtT, nxt = nxtT, nxt, curT, cur

    def norm_to_yT(o_ps, blk, t_, eps_i, center, gate=None, scale_bc=None, bias_bc=None, add=None, tmp=None, width=128, gate2=None):
        o = tmp
        if center:
            P.rsum(smc(24), o_ps)
            P.ts("dve", smc(25), smc(24), -1.0 / width, None, ALU.mult)
            P.act(o, o_ps, AF.Identity, bias=small[:, 25:26], extra=[smc(25)])
        else:
            P.copy("act", o, o_ps)
        P.act(W(junk, junk[:, 0:width]), o, AF.Square, accum=smc(26))
        rsqrt_col(27, 26, eps_i, 1.0 / width)
        if scale_bc is not None:
            P.stt(o, o, small[:, 27:28], scale_bc, ALU.mult, ALU.mult, extra=[smc(27)])
        else:
            P.ts("dve", o, o, small[:, 27:28], None, ALU.mult, extra=[smc(27)])
        if bias_bc is not None:
            P.tt("dve", o, o, bias_bc, ALU.add)
        if add is not None:
            P.tt("dve", o, o, add, ALU.add)
        if gate is not None:
            P.tt("dve", o, o, gate, ALU.mult)
        return o

    def to_yT(o, blk, t_):
        p_ = ps()
        P.tr(V(p_, p_[:, 0:128]), o, ident)
        P.copy(ev_eng(), W(yT, yT[:, blk, t_ * 128:(t_ + 1) * 128], blk), V(p_, p_[:, 0:128]))

    def retention(l, sq, tok0):
        P.dma("sp", posi[:], pos_d[sq:sq + 1, tok0:tok0 + NT].to_broadcast([128, NT]), [], [W(posi)])
        posf = Sl(18, NT)
        P.copy("dve", posf, V(posi, posi[:].bitcast(I32)))
        for which, slot in ((0, 16), (1, 17)):
            ang = Sl(19, NT)
            if which == 0:
                P.ts("dve", ang, posf, cst[:, 7, 0:1], math.pi / 2, ALU.mult, ALU.add, extra=[C(7)])
            else:
                P.ts("dve", ang, posf, cst[:, 7, 0:1], None, ALU.mult, extra=[C(7)])
            nf = Sl(slot, NT)
            P.ts("dve", nf, ang, 1.0 / TWO_PI, MAGIC, ALU.mult, ALU.add)
            P.ts("dve", nf, nf, -MAGIC, None, ALU.add)
            P.stt(ang, nf, -C1, ang, ALU.mult, ALU.add)
            P.stt(ang, nf, -C2, ang, ALU.mult, ALU.add)
            P.act(nf, ang, AF.Sin)
        cosT, sinT = Sl(16, NT), Sl(17, NT)
        if "ret1" in skip:
            return
        for h in range(4):
            wbuf = next_wb()
            wv_ = None
            for i_, c0 in enumerate((RQ, RK, RV, RG)):
                wv_ = load_w(wbuf, w_in_d[l][:, c0 + h * 128: c0 + (h + 1) * 128], D, 128, col_off=i_ * 128, tot=512)
            qk = []
            for j in range(2):
                p_ = lin_W(wbuf, wv_, j, xT, KC, NT)
                raw = Sl(j, NT)
                P.copy("act", raw, V(p_, p_[:, 0:NT]))
                p2 = ps()
                P.mm(V(p2, p2[:, 0:NT]), psw, raw, True, True)
                rot = Sl(2 + j, NT)
                P.tt("dve", rot, raw, cosT, ALU.mult)
                P.tt("dve", raw, V(p2, p2[:, 0:NT]), sinT, ALU.mult)
                P.tt("dve", rot, rot, raw, ALU.add)
                dec = cst[:, (8 if j == 0 else 12) + h, :]
                for c in range(NTT):
                    rv = V(SC, SC[:, 2 + j, c * 128:(c + 1) * 128], [(2 + j) * 4 + c])
                    P.tt("dve", rv, rv, V(cst, dec), ALU.mult)
                qk.append(2 + j)
            if "ret2" in skip:
                continue
            vtm, gtm_ = 4, 5
            for t_ in range(NTT):
                p_ = lin_X(wbuf, wv_, 256, 256, xT, KC, t_)
                P.copy("dve", Sq(vtm, t_), V(p_, p_[:, 0:128]))
                P.act(Sq(gtm_, t_), V(p_, p_[:, 128:256]), AF.Sigmoid)
                P.tt("dve", Sq(gtm_, t_), Sq(gtm_, t_), V(p_, p_[:, 128:256]), ALU.mult)
            R = W(S_r, S_r[:, l * 4 + h, :], l * 4 + h)
            gC = RET_GAMMA[h] ** 128
            if "ret3" in skip:
                continue
            for c in range(NTT):
                qc, kc_ = Sq(2, c), Sq(3, c)
                p_ = ps()
                P.mm(V(p_, p_[:, 0:128]), kc_, qc, True, True)
                PT = Sq(6, 0)
                P.tt("dve", PT, V(p_, p_[:, 0:128]), mincl, ALU.mult)
                po = ps()
                P.mm(V(po, po[:, 0:128]), PT, Sq(vtm, c), True, False)
                P.mm(V(po, po[:, 0:128]), qc, R, False, True)
                pk = ps()
                P.tr(V(pk, pk[:, 0:128]), kc_, ident)
                ktm = Sq(6, 1)
                P.copy("act", ktm, V(pk, pk[:, 0:128]))
                pr = ps()
                P.mm(V(pr, pr[:, 0:128]), ktm, Sq(vtm, c), True, True)
                o = norm_to_yT(V(po, po[:, 0:128]), 6 + h, c, 1, True, gate=Sq(gtm_, c), tmp=Sq(6, 2))
                to_yT(o, 6 + h, c)
                P.tt("dve", R, R, V(pr, pr[:, 0:128]), ALU.add)
                P.ts("dve", R, R, gC, None, ALU.mult)

    def gdn(l, sq, first):
        wbuf = next_wb()
        wv_ = load_w(wbuf, w_in_d[l][:, GB:GB + 12], D, 12)
        for t_ in range(NTT):
            p_ = lin_X(wbuf, wv_, 0, 12, xT, KC, t_)
            bt = W(gtm, gtm[:, t_, 0:6], t_)
            la = W(gtm, gtm[:, t_, 6:12], t_)
            P.act(bt, V(p_, p_[:, 0:6]), AF.Sigmoid)
            P.tt("dve", la, V(p_, p_[:, 6:12]), W(gsm, gsm[:, 6:12]), ALU.add)
            P.act(la, la, AF.Exp)
            P.act(la, la, AF.Ln, bias=small[:, 6:7], extra=[smc(6)])
            P.tt("dve", la, la, W(gsm, gsm[:, 0:6]), ALU.mult)
            pl = ps()
            P.mm(V(pl, pl[:, 0:6]), tri, la, True, True)
            P.copy("dve", W(gL, gL[:, t_, 0:6], t_), V(pl, pl[:, 0:6]))
            P.act(W(gL, gL[:, t_, 6:12], t_), V(pl, pl[:, 0:6]), AF.Exp)
        for h in range(6):
            wbuf = next_wb()
            wv_ = None
            for i_, c0 in enumerate((GQ, GK, GV, GG)):
                wv_ = load_w(wbuf, w_in_d[l][:, c0 + h * 128: c0 + (h + 1) * 128], D, 128, col_off=i_ * 128, tot=512)
            for j in range(3):
                blk = j * 6 + h
                cb_ = W(cbuf, cbuf[:, j, :], j)
                P.copy("dve", W(cbuf, cbuf[:, j, 0:3], j), W(cv_c, cv_c[:, l * 18 + blk, :], l * 18 + blk))
                p_ = lin_W(wbuf, wv_, j, xT, KC, NT)
                P.copy("act", W(cbuf, cbuf[:, j, 3:3 + NT], j), V(p_, p_[:, 0:NT]))
                P.copy("dve", W(cv_c, cv_c[:, l * 18 + blk, :], l * 18 + blk), W(cbuf, cbuf[:, j, NT:NT + 3], j))
                acc = Sl(j, NT)
                P.ts("dve", acc, W(cbuf, cbuf[:, j, 3:3 + NT], j), convw[:, blk * 4 + 3:blk * 4 + 4], None, ALU.mult, extra=[W(convw)])
                for jj in range(3):
                    P.stt(acc, W(cbuf, cbuf[:, j, jj:jj + NT], j), convw[:, blk * 4 + jj:blk * 4 + jj + 1], acc, ALU.mult, ALU.add, extra=[W(convw)])
                P.act(Sl(3, NT), acc, AF.Sigmoid)
                P.tt("dve", acc, acc, Sl(3, NT), ALU.mult)
                if j < 2:
                    sqv = Sl(3, NT)
                    P.tt("dve", sqv, acc, acc, ALU.mult)
                    p2 = ps()
                    P.mm(V(p2, p2[:, 0:NT]), ones, sqv, True, True)
                    P.act(sqv, V(p2, p2[:, 0:NT]), AF.Sqrt, bias=small[:, 1:2], extra=[smc(1)])
                    P.recip(sqv, sqv)
                    if j == 0:
                        P.stt(acc, acc, 128.0 ** -0.5, sqv, ALU.mult, ALU.mult)
                    else:
                        P.tt("dve", acc, acc, sqv, ALU.mult)
            for t_ in range(NTT):
                p_ = lin_X(wbuf, wv_, 384, 128, xT, KC, t_)
                P.act(Sq(4, t_), V(p_, p_[:, 0:128]), AF.Sigmoid)
                P.tt("dve", Sq(4, t_), Sq(4, t_), V(p_, p_[:, 0:128]), ALU.mult)
            S = W(S_g, S_g[:, l * 6 + h, :], l * 6 + h)
            for c in range(NTT):
                qc, kc_, vc = Sq(0, c), Sq(1, c), Sq(2, c)
                bcol = gtm[:, c, h:h + 1]
                Lcol = gL[:, c, h:h + 1]
                eLcol = gL[:, c, 6 + h:7 + h]
                gk_ = [W(gtm, None, c), W(gL, None, c)]
                pk = ps()
                P.tr(V(pk, pk[:, 0:128]), kc_, ident)
                pv = ps()
                P.tr(V(pv, pv[:, 0:128]), vc, ident)
                ktm = Sq(5, 0)
                P.copy("act", ktm, V(pk, pk[:, 0:128]))
                Y = Sq(6, 0, 2)
                P.copy("act", Sq(6, 0), V(pv, pv[:, 0:128]))
                P.ts("dve", Sq(6, 1), V(pk, pk[:, 0:128]), eLcol, None, ALU.mult, extra=gk_)
                plb = ps()
                P.mm(V(plb, plb[:, 0:128]), W(gtm, gtm[:, c, 6 + h:7 + h].to_broadcast([128, 128]), c), tri, True, True)
                Ei = Sq(5, 1)
                P.ts("dve", Ei, V(plb, plb[:, 0:128]), Lcol, 0.0, ALU.subtract, ALU.min, extra=gk_)
                P.act(Ei, Ei, AF.Exp)
                ebc = Sq(5, 2)
                P.act(ebc, V(plb, plb[:, 0:128]), AF.Exp)
                P.tt("dve", smc(30), W(gtm, bcol, c), V(SC, SC[:, 5, 128 + 127:128 + 128], [5 * 4 + 1]), ALU.mult)
                P.ts("dve", smc(31), W(gtm, bcol, c), -1.0, None, ALU.mult)
                Eis = Sq(5, 3)
                P.tt("dve", Eis, Ei, mstrict, ALU.mult)
                P.tt("dve", Ei, Ei, mincl, ALU.mult)
                pg = ps()
                P.mm(V(pg, pg[:, 0:128]), kc_, kc_, True, True)
                P.mm(V(pg, pg[:, 128:256]), kc_, qc, True, True)
                XT = Sq(7, 0)
                P.stt(XT, V(pg, pg[:, 0:128]), small[:, 31:32], Eis, ALU.mult, ALU.mult, extra=[smc(31)])
                PT = Sq(7, 1)
                P.stt(PT, V(pg, pg[:, 128:256]), bcol, Ei, ALU.mult, ALU.mult, extra=gk_)
                px = ps()
                P.tr(V(px, px[:, 0:128]), XT, ident)
                X = Sq(7, 2)
                P.copy("act", X, V(px, px[:, 0:128]))
                neumann(XT, X, Y, Sq(7, 3), Sq(8, 0), 256)
                pw = ps()
                P.tr(V(pw, pw[:, 0:128]), Sq(6, 1), ident)
                WT = Sq(8, 1)
                P.copy("act", WT, V(pw, pw[:, 0:128]))
                qe = Sq(8, 2)
                P.tt("dve", qe, qc, ebc, ALU.mult)
                kd = Sq(8, 3)
                P.ts("dve", kd, ktm, small[:, 30:31], None, ALU.mult, extra=[smc(30)])
                p1 = ps()
                P.mm(V(p1, p1[:, 0:128]), WT, S, True, True)
                Vn = Sq(9, 0)
                P.tt("dve", Vn, Sq(6, 0), V(p1, p1[:, 0:128]), ALU.subtract)
                po = ps()
                P.mm(V(po, po[:, 0:128]), qe, S, True, False)
                P.mm(V(po, po[:, 0:128]), PT, Vn, False, True)
                p3 = ps()
                P.mm(V(p3, p3[:, 0:128]), kd, Vn, True, True)
                P.stt(S, S, SC[:, 5, 2 * 128 + 127:2 * 128 + 128], V(p3, p3[:, 0:128]), ALU.mult, ALU.add, extra=[ebc])
                o = norm_to_yT(V(po, po[:, 0:128]), h, c, 1, False, gate=Sq(4, c), scale_bc=W(gnorm), tmp=Sq(9, 1))
                to_yT(o, h, c)

    def rwkv(l, sq):
        NEG = -math.exp(-0.5)
        lnxw = V(SC, SC[:, 16:19, :].rearrange("p a f -> p (a f)")[:, 0:768], list(range(64, 76)))
        lnxb_ap = SC[:, 16:19, :].rearrange("p a f -> p (a f)")[:, 768:1536]
        lk = list(range(64, 76))
        P.dma("sp", SC[:, 16:19, :].rearrange("p a f -> p (a f)")[:, 0:1536], lnx_d[l:l + 1, :].to_broadcast([128, 1536]), [], [V(SC, None, lk)])
        lnx_all = V(SC, None, lk)

        def shifted_block(colblk, c0, ncols, dst, tsidx, pre=None):
            raise NotImplementedError

        def proj_shift(wbuf, wv_, j, blk, dst):
            p_ = lin_W(wbuf, wv_, j, xT, KC, NT)
            cb_ = W(cbuf, cbuf[:, 0, :], 0)
            idx = l * 20 + blk
            P.copy("dve", W(cbuf, cbuf[:, 0, 0:1], 0), W(ts_c, ts_c[:, idx:idx + 1], idx))
            P.copy("act", W(cbuf, cbuf[:, 0, 1:1 + NT], 0), V(p_, p_[:, 0:NT]))
            P.copy("dve", W(ts_c, ts_c[:, idx:idx + 1], idx), W(cbuf, cbuf[:, 0, NT:NT + 1], 0))
            dlt = Sl(15, NT)
            P.tt("dve", dlt, W(cbuf, cbuf[:, 0, 0:NT], 0), W(cbuf, cbuf[:, 0, 1:1 + NT], 0), ALU.subtract)
            P.stt(dst, dlt, mu[:, blk:blk + 1], W(cbuf, cbuf[:, 0, 1:1 + NT], 0), ALU.mult, ALU.add, extra=[W(mu)])

        wbuf = next_wb()
        wv_ = load_w(wbuf, w_in_d[l][:, WWD:WWD + 256], D, 256)
        lora = Sl(13, NT)
        sgd = Sl(14, NT)
        proj_shift(wbuf, wv_, 0, 18, lora)
        P.act(V(SC, SC[0:64, 13, 0:NT], [52, 53, 54, 55]), V(SC, SC[0:64, 13, 0:NT], [52, 53, 54, 55]), AF.Tanh)
        proj_shift(wbuf, wv_, 1, 19, sgd)
        P.act(sgd, sgd, AF.Sigmoid)
        for hp in range(6):
            wbuf = next_wb()
            wv_ = None
            for i_, c0 in enumerate((WR, WK, WV)):
                wv_ = load_w(wbuf, w_in_d[l][:, c0 + hp * 128: c0 + (hp + 1) * 128], D, 128, col_off=i_ * 128, tot=384)
            rT, kT_, vT_ = Sl(0, NT), Sl(1, NT), Sl(2, NT)
            proj_shift(wbuf, wv_, 0, hp, rT)
            proj_shift(wbuf, wv_, 1, 6 + hp, kT_)
            proj_shift(wbuf, wv_, 2, 12 + hp, vT_)
            pz = ps()
            P.mm(V(pz, pz[:, 0:NT]), W(wup, wup[0:64, hp * 128:(hp + 1) * 128]), V(SC, SC[0:64, 13, 0:NT], [52, 53, 54, 55]), True, True)
            logw = Sl(3, NT)
            P.act(logw, V(pz, pz[:, 0:NT]), AF.Sigmoid, bias=rvec[:, 0 * 6 + hp:0 * 6 + hp + 1], extra=[W(rvec)])
            P.ts("dve", logw, logw, NEG, None, ALU.mult)
            pa = ps()
            P.mm(V(pa, pa[:, 0:NT]), W(aup, aup[64:128, hp * 128:(hp + 1) * 128]), V(SC, SC[64:128, 13, 0:NT], [52, 53, 54, 55]), True, True)
            aT = Sl(4, NT)
            P.act(aT, V(pa, pa[:, 0:NT]), AF.Sigmoid, bias=rvec[:, 1 * 6 + hp:1 * 6 + hp + 1], extra=[W(rvec)])
            kk = Sl(5, NT)
            P.ts("dve", kk, kT_, rvec[:, 2 * 6 + hp:2 * 6 + hp + 1], None, ALU.mult, extra=[W(rvec)])
            tmp = Sl(6, NT)
            P.tt("dve", tmp, kk, kk, ALU.mult)
            p2 = ps()
            P.mm(V(p2, p2[:, 0:NT]), bones, tmp, True, True)
            P.act(tmp, V(p2, p2[:, 0:NT]), AF.Sqrt, bias=small[:, 2:3], extra=[smc(2)])
            P.recip(tmp, tmp)
            P.tt("dve", kk, kk, tmp, ALU.mult)
            P.ts("dve", tmp, aT, -1.0, rvec[:, 3 * 6 + hp:3 * 6 + hp + 1], ALU.add, ALU.mult, extra=[W(rvec)])
            P.stt(kT_, tmp, 1.0, kT_, ALU.add, ALU.mult)
            P.tt("dve", aT, kk, aT, ALU.mult)
            P.ts("dve", kk, kk, -1.0, None, ALU.mult)
            P.stt(tmp, rT, rvec[:, 4 * 6 + hp:4 * 6 + hp + 1], kT_, ALU.mult, ALU.mult, extra=[W(rvec)])
            for c in range(NTT):
                def q(slot):
                    return Sq(slot, c)
                pt = ps()
                P.tr(V(pt, pt[:, 0:128]), q(3), ident)
                lwtm = Sq(7, 0)
                P.copy("act", lwtm, V(pt, pt[:, 0:128]))
                pL = ps()
                P.mm(V(pL, pL[:, 0:128]), lwtm, tri, True, True)
                L = Sq(7, 1)
                P.copy("dve", L, V(pL, pL[:, 0:128]))
                LC = SC[:, 7, 128 + 127:128 + 128]
                e = Sq(7, 2)
                fm = Sq(8, 0, 4)
                P.act(e, L, AF.Exp)
                P.tt("dve", Sq(8, 1), q(0), e, ALU.mult)
                P.copy("dve", smc(32), V(SC, SC[:, 7, 2 * 128 + 127:2 * 128 + 128], [7 * 4 + 2]))
                P.tt("dve", e, L, q(3), ALU.subtract)
                P.act(e, e, AF.Exp)
                P.tt("dve", Sq(8, 0), q(5), e, ALU.mult)
                P.act(e, L, AF.Exp, scale=-1.0)
                P.tt("dve", Sq(8, 2), q(4), e, ALU.mult)
                P.tt("dve", Sq(8, 3), q(1), e, ALU.mult)
                P.act(e, L, AF.Exp, scale=-1.0, bias=LC, extra=[L])
                P.tt("dve", Sq(9, 0), q(4), e, ALU.mult)
                P.tt("dve", Sq(9, 1), q(1), e, ALU.mult)
                for qi, src in enumerate((Sq(8, 0), Sq(9, 0), Sq(9, 1), q(2))):
                    p_ = ps()
                    P.tr(V(p_, p_[:, 0:128]), src, ident)
                    P.copy(ev_eng(), Sq(10, qi), V(p_, p_[:, 0:128]))
                pb = ps()
                P.mm(V(pb, pb[:, 0:2]), q(6), V(cst, cst[:, 7, 1:3]), True, True)
                P.copy("dve", V(small, small[:, 34:36], [34, 35]), V(pb, pb[:, 0:2]))
                pgt = ps()
                P.mm(V(pgt, pgt[:, 0:128]), Sq(14, c), W(gup, gup[:, hp * 128:(hp + 1) * 128]), True, True)
                gt = Sq(7, 3)
                P.copy("act", gt, V(pgt, pgt[:, 0:128]))
                yo = Sq(9, 3)
                Wt = Sq(9, 2)
                hd = []
                for hh in range(2):
                    p0, p1_ = hh * 64, (hh + 1) * 64

                    def fmv(qi, p0=p0, p1_=p1_):
                        return V(SC, SC[p0:p1_, 8, qi * 128:(qi + 1) * 128], [8 * 4 + qi])
                    ar = V(SC, SC[p0:p1_, 8, 0:256], [32, 33])
                    pgb = ps()
                    P.mm(V(pgb, pgb[:, 0:256]), fmv(2), ar, True, True)
                    pgk = ps()
                    P.mm(V(pgk, pgk[:, 0:256]), fmv(3), ar, True, True)
                    sl_ = 11 + hh
                    XT, RbT, AkT, RkT = Sq(sl_, 0), Sq(sl_, 1), Sq(sl_, 2), Sq(sl_, 3)
                    P.tt("dve", XT, V(pgb, pgb[:, 0:128]), mstrict, ALU.mult)
                    P.tt("dve", RbT, V(pgb, pgb[:, 128:256]), mincl, ALU.mult)
                    P.tt("dve", AkT, V(pgk, pgk[:, 0:128]), mstrict, ALU.mult)
                    P.tt("dve", RkT, V(pgk, pgk[:, 128:256]), mincl, ALU.mult)
                    px = ps()
                    P.tr(V(px, px[:, 0:128]), XT, ident)
                    X = Sq(19, 0)
                    P.copy("act", X, V(px, px[:, 0:128]))
                    Vh = V(SC, SC[:, 10, 3 * 128 + p0:3 * 128 + p1_], [43])
                    yq = 1 + hh
                    Y = Sq(15, yq)
                    pav = ps()
                    P.mm(V(pav, pav[:, 0:64]), AkT, Vh, True, True)
                    P.copy("act", V(SC, SC[:, 15, yq * 128:yq * 128 + 64], [60 + yq]), V(pav, pav[:, 0:64]))
                    P.copy("dve", V(SC, SC[:, 15, yq * 128 + 64:yq * 128 + 128], [60 + yq]), V(SC, SC[:, 10, p0:p1_], [40]))
                    neumann(XT, X, Y, Sq(19, 1), Sq(19, 2), 128)
                    P.copy("dve", V(SC, SC[:, 9, 2 * 128 + p0:2 * 128 + p1_], [38]), V(SC, SC[:, 15, yq * 128 + 64:yq * 128 + 128], [60 + yq]))
                    hd.append((p0, p1_, fmv, RbT, RkT, Vh, yq))
                pw = ps()
                P.tr(V(pw, pw[:, 0:128]), Wt, ident)
                W1T = Sq(15, 3)
                P.copy("act", W1T, V(pw, pw[:, 0:128]))
                for hh in range(2):
                    p0, p1_, fmv, RbT, RkT, Vh, yq = hd[hh]
                    Hh = W(S_w, S_w[p0:p1_, l * 6 + hp, :], l * 6 + hp)
                    pu = ps()
                    P.mm(V(pu, pu[:, 0:64]), V(SC, SC[p0:p1_, 15, 3 * 128:4 * 128], [63]), Hh, True, True)
                    U = V(SC, SC[:, 15, hh * 64:(hh + 1) * 64], [60])
                    P.tt("dve", U, V(SC, SC[:, 15, yq * 128:yq * 128 + 64], [60 + yq]), V(pu, pu[:, 0:64]), ALU.add)
                    py = ps()
                    P.mm(V(py, py[:, 0:64]), fmv(1), Hh, True, False)
                    P.mm(V(py, py[:, 0:64]), RbT, U, False, False)
                    P.mm(V(py, py[:, 0:64]), RkT, Vh, False, True)
                    ph = ps()
                    P.mm(V(ph, ph[:, 0:64]), Sq(10, 1), U, True, False)
                    P.mm(V(ph, ph[:, 0:64]), Sq(10, 2), Vh, False, True)
                    P.stt(Hh, Hh, small[p0:p1_, 32:33], V(ph, ph[p0:p1_, 0:64]), ALU.mult, ALU.add, extra=[smc(32)])
                    yh = V(SC, SC[:, 9, 3 * 128 + p0:3 * 128 + p1_], [39])
                    bon = V(SC, SC[:, 19, 3 * 128:3 * 128 + 64], [79])
                    P.ts("dve", bon, Vh, small[:, 34 + hh:35 + hh], None, ALU.mult, extra=[smc(34 + hh)])
                    f0 = hp * 128 + p0
                    norm_to_yT(V(py, py[:, 0:64]), None, c, 3, True,
                               scale_bc=V(SC, SC[:, 16:19, :].rearrange("p a f -> p (a f)")[:, f0:f0 + 64], lk),
                               bias_bc=V(SC, lnxb_ap[:, f0:f0 + 64], lk), add=bon, tmp=yh, width=64)
                P.tt("dve", yo, yo, gt, ALU.mult)
                to_yT(yo, 10 + hp, c)

    def mixer(l, sq, tok0):
        if "ret" not in skip:
            retention(l, sq, tok0)
        P.barrier()
        if "gdn" not in skip:
            gdn(l, sq, tok0 == 0)
        P.barrier()
        if "rwkv" not in skip:
            rwkv(l, sq)

    def load_layer_params(l):
        P.dma("sp", convw[:], conv_d[l], [], [W(convw)])
        P.dma("sp", gsm[:], gsm_d[l:l + 1, :].to_broadcast([128, 12]), [], [W(gsm)])
        P.dma("sp", gnorm[:], gnorm_d[l:l + 1, :].to_broadcast([128, 128]), [], [W(gnorm)])
        P.dma("sp", mu[:], mu_d[l], [], [W(mu)])
        P.dma("sp", rvec[:], rvec_d[l], [], [W(rvec)])
        P.dma("sp", wup[0:64, :], wup_d[l], [], [W(wup)])
        P.dma("sp", aup[64:128, :], aup_d[l], [], [W(aup)])
        P.dma("sp", gup[:], gup_d[l], [], [W(gup)])
        P.act(W(gsm, gsm[:, 0:6]), W(gsm, gsm[:, 0:6]), AF.Exp)
        P.ts("dve", W(gsm, gsm[:, 0:6]), W(gsm, gsm[:, 0:6]), -1.0, None, ALU.mult)

    P.dma("sp", cst[:], cst_d, [], [W(cst)])
    for i_, v_ in ((0, 1e-5), (1, 1e-6), (2, 1e-12), (3, 64e-5), (4, 0.0), (6, 1.0)):
        P.memset("dve", smc(i_), v_)
    P.memset("dve", W(onesb), 1.0)
    P.barrier()

    for sq in range(nseq):
        st_views = [(S_g, S_g[:].rearrange("p a b -> p (a b)"), depth * 768), (S_r, S_r[:].rearrange("p a b -> p (a b)"), depth * 512),
                    (S_w, S_w[:].rearrange("p a b -> p (a b)"), depth * 384), (cv_c, cv_c[:].rearrange("p a b -> p (a b)"), depth * 54),
                    (ts_c, ts_c[:], depth * 20)]
        if state_io:
            o_ = 0
            for b_, v_, n_ in st_views:
                P.dma("sp", v_, stin_d[sq, :, o_:o_ + n_], [], [W(b_)])
                o_ += n_
        else:
            for b_ in (S_g, S_r, S_w, cv_c, ts_c):
                P.memset("dve", W(b_), 0.0)
        for st_i in range(NST):
            tok0 = st_i * NT
            for t_ in range(NTT):
                P.dma("sp", xres[:, t_, :], x_d[sq, tok0 + t_ * 128: tok0 + (t_ + 1) * 128, :], [], [W(xres, None, t_)])
            for l in range(depth):
                P.barrier()
                load_layer_params(l)
                make_T(lambda t_, c0, c1: W(xres, xres[:, t_, c0:c1], t_), xT, NTT, D)
                mixer(l, sq, tok0)
                P.barrier()
                tap(f"yT_{l}", W(yT), [128, KC, NT])
                if "w_out" not in skip:
                    sublayer_out(w_out_d[l], l, 0)
                P.barrier()
                tap(f"x1_{l}", W(xres), [128, NTT, D])
                make_T(lambda t_, c0, c1: W(xres, xres[:, t_, c0:c1], t_), xT, NTT, D)
                if "xattn" not in skip:
                    xattn(l, sq)
                    P.barrier()
                    sublayer_out(wo_d[l], l, 1)
                P.barrier()
                tap(f"x2_{l}", W(xres), [128, NTT, D])
                make_T(lambda t_, c0, c1: W(xres, xres[:, t_, c0:c1], t_), xT, NTT, D)
                if "ffn" not in skip:
                    ffn(l)
                P.barrier()
            for t_ in range(NTT):
                P.dma("sp", out_d[sq, tok0 + t_ * 128: tok0 + (t_ + 1) * 128, :], xres[:, t_, :], [W(xres, None, t_)], [])
        if state_io:
            P.barrier()
            o_ = 0
            for b_, v_, n_ in st_views:
                P.dma("sp", stout_d[sq, :, o_:o_ + n_], v_, [W(b_)], [])
                o_ += n_
    P.finish()
    return P


def prep_params(inp, depth=DEPTH, layer=None):
    f = lambda a: np.ascontiguousarray(np.asarray(a, dtype=np.float32))
    if layer is not None:
        inp = {k: (np.asarray(v)[layer:layer + 1] if k not in ("x", "mem", "positions") else v) for k, v in inp.items()}
        depth = 1
    d = {}
    for k in ("w_in", "w_out", "xattn_q", "xattn_k", "xattn_v", "xattn_o", "ffn_gate_up", "ffn_down",
              "rwkv_w_up", "rwkv_a_up", "rwkv_g_up", "gdn_norm"):
        d[k] = f(inp[k])[:depth]
    d["lnp"] = f(np.stack([inp["ln1_g"], inp["ln1_b"], inp["ln2_g"], inp["ln2_b"], inp["ln3_g"], inp["ln3_b"]], axis=1))[:depth]
    cv = f(inp["gdn_conv"])[:depth]
    d["gdn_conv_t"] = np.ascontiguousarray(cv.reshape(depth, 4, 18, 128).transpose(0, 3, 2, 1).reshape(depth, 128, 72))
    d["gdn_small"] = f(np.concatenate([inp["gdn_a_log"], inp["gdn_dt_bias"]], axis=1))[:depth]
    m = f(inp["rwkv_mu"])[:depth]
    d["rwkv_mu_t"] = np.ascontiguousarray(m.reshape(depth, 20, 128).transpose(0, 2, 1))
    rv = np.stack([f(inp[k])[:depth] for k in ("rwkv_w0", "rwkv_a0", "rwkv_k_k", "rwkv_k_a", "rwkv_r_k")], axis=1)
    d["rwkv_vec_t"] = np.ascontiguousarray(rv.reshape(depth, 5, 6, 128).transpose(0, 3, 1, 2).reshape(depth, 128, 30))
    d["rwkv_lnx"] = f(np.concatenate([inp["rwkv_lnx_w"], inp["rwkv_lnx_b"]], axis=1))[:depth]
    d["consts"] = make_consts()
    return d


NCORES = 8


def kernel(**inputs):
    x = np.asarray(inputs["x"], dtype=np.float32)
    mem = np.asarray(inputs["mem"], dtype=np.float32)
    pos = np.asarray(inputs["positions"], dtype=np.int32)
    B, T, _ = x.shape
    nseq = B // NCORES
    NT = 512
    P = build(nseq, NT, NT, 1, state_io=True)
    pars = [prep_params(inputs, layer=l) for l in range(DEPTH)]
    states = [[np.zeros((nseq, 128, NSTATE), np.float32) for _ in range(NCORES)] for _ in range(DEPTH)]
    out = np.empty_like(x)
    for i in range(T // NT):
        cur = [np.ascontiguousarray(x[c * nseq:(c + 1) * nseq, i * NT:(i + 1) * NT]) for c in range(NCORES)]
        for l in range(DEPTH):
            in_maps = []
            for c in range(NCORES):
                m = dict(pars[l])
                m["x"] = cur[c]
                m["mem"] = np.ascontiguousarray(mem[c * nseq:(c + 1) * nseq])
                m["positions"] = np.ascontiguousarray(pos[c * nseq:(c + 1) * nseq, i * NT:(i + 1) * NT]).view(np.float32)
                m["state_in"] = states[l][c]
                in_maps.append(m)
            res = run_bass_kernel_spmd(P.nc, in_maps, core_ids=list(range(NCORES)))
            cur = [np.ascontiguousarray(r["out"]) for r in res.results]
            states[l] = [np.ascontiguousarray(r["state_out"]) for r in res.results]
        for c in range(NCORES):
            out[c * nseq:(c + 1) * nseq, i * NT:(i + 1) * NT] = cur[c]
    return out
```
